# Optimizing a Trainium2 kernel written in Bass

```python
import math
import jax, jax.numpy as jnp
from jax import lax
import numpy as np

D_MODEL = 2048
BATCH = 1
SEQ = 8192
DEPTH = 2

N_META = 16
MIX_WIDTH = D_MODEL
HEAD_SIZE = 64
RWKV_DIM = MIX_WIDTH // 2
RWKV_HEADS = RWKV_DIM // HEAD_SIZE
CONV_DIM = MIX_WIDTH - RWKV_DIM
N_DIR = 2
DECAY_LORA = 64
ICLR_LORA = 64
GATE_LORA = 128
RWKV_COLS = 3 * RWKV_DIM + N_DIR * DECAY_LORA + N_DIR * ICLR_LORA + GATE_LORA
CONV_COLS = 3 * CONV_DIM
IN_COLS = RWKV_COLS + CONV_COLS
CONV_WIDTH = 3
N_EXPERTS = 32
TOP_K = 4
D_EXPERT = D_MODEL
SWIGLU_LIMIT = 7.0
SWIGLU_ALPHA = 1.702
MOE_BLOCK = 128
ALPHA_RES = (2 * DEPTH) ** 0.25
BETA_INIT = (8 * DEPTH) ** -0.25
LN_EPS = 1e-5
GN_EPS = 64e-5

kernel_name = "hybrid_rwkv7_shortconv_moe_encoder"


def layer_norm(x, g, b):
    xf = x.astype(jnp.float32)
    mu = jnp.mean(xf, axis=-1, keepdims=True)
    var = jnp.mean(jnp.square(xf - mu), axis=-1, keepdims=True)
    y = (xf - mu) * lax.rsqrt(var + LN_EPS) * g.astype(jnp.float32) + b.astype(jnp.float32)
    return y.astype(x.dtype)


def shift_prev(u):
    return jnp.pad(u, ((0, 0), (1, 0), (0, 0)))[:, :-1]


def shift_next(u):
    return jnp.pad(u, ((0, 0), (0, 1), (0, 0)))[:, 1:]


def rwkv7_bidir_scan(r, decay, k, v, a_vec, b_vec):
    xs = tuple(jnp.moveaxis(t, 1, 0) for t in (r, decay, k, v, a_vec, b_vec))
    bsz = r.shape[0]
    s0 = jnp.zeros((bsz, N_DIR, RWKV_HEADS, HEAD_SIZE, HEAD_SIZE), jnp.float32)

    def step(S, inp):
        r_t, w_t, k_t, v_t, a_t, b_t = inp
        sa = jnp.einsum('bdhij,bdhj->bdhi', S, a_t)
        S = S * w_t[..., None, :] + sa[..., :, None] * b_t[..., None, :] + v_t[..., :, None] * k_t[..., None, :]
        y = jnp.einsum('bdhij,bdhj->bdhi', S, r_t)
        return S, y

    _, ys = lax.scan(step, s0, xs)
    return jnp.moveaxis(ys, 0, 1)


def mixer(h, w_in, mu_shift, decay0, decay_up, iclr0, iclr_up, gate_up, k_k, k_a, r_k,
          lnx_g, lnx_b, conv_w, out_scale, w_out):
    bsz, seqlen = h.shape[0], h.shape[1]
    p = h @ w_in

    R = p[..., :RWKV_COLS].astype(jnp.float32)
    R = R + (0.5 * (shift_prev(R) + shift_next(R)) - R) * mu_shift
    r = R[..., :RWKV_DIM]
    k = R[..., RWKV_DIM:2 * RWKV_DIM]
    v = R[..., 2 * RWKV_DIM:3 * RWKV_DIM]
    o = 3 * RWKV_DIM
    cw = R[..., o:o + N_DIR * DECAY_LORA].reshape(bsz, seqlen, N_DIR, DECAY_LORA)
    o += N_DIR * DECAY_LORA
    ca = R[..., o:o + N_DIR * ICLR_LORA].reshape(bsz, seqlen, N_DIR, ICLR_LORA)
    o += N_DIR * ICLR_LORA
    cg = R[..., o:o + GATE_LORA]

    w_log = -jax.nn.softplus(-(decay0 + jnp.einsum('bldr,drc->bldc', jnp.tanh(cw), decay_up))) - 0.5
    decay = jnp.exp(-jnp.exp(w_log))
    a = jax.nn.sigmoid(iclr0 + jnp.einsum('bldr,drc->bldc', ca, iclr_up))
    g = jax.nn.sigmoid(cg) @ gate_up

    heads = lambda t: t.reshape(t.shape[:-1] + (RWKV_HEADS, HEAD_SIZE))
    kk = heads(k * k_k)
    kk = kk / jnp.maximum(jnp.linalg.norm(kk, axis=-1, keepdims=True), 1e-12)
    kk = kk.reshape(bsz, seqlen, RWKV_DIM)
    k_dir = k[:, :, None, :] * (1.0 + (a - 1.0) * k_a)
    b_dir = kk[:, :, None, :] * a

    def to_dirs(f, bw):
        return heads(jnp.stack([f, jnp.flip(bw, axis=1)], axis=2))

    ys = rwkv7_bidir_scan(
        to_dirs(r, r), to_dirs(decay[:, :, 0], decay[:, :, 1]),
        to_dirs(k_dir[:, :, 0], k_dir[:, :, 1]), to_dirs(v, v),
        to_dirs(-kk, -kk), to_dirs(b_dir[:, :, 0], b_dir[:, :, 1]))
    y = ys[:, :, 0] + jnp.flip(ys[:, :, 1], axis=1)

    mu = jnp.mean(y, axis=-1, keepdims=True)
    var = jnp.mean(jnp.square(y - mu), axis=-1, keepdims=True)
    y = (y - mu) * lax.rsqrt(var + GN_EPS) * heads(lnx_g) + heads(lnx_b)
    rh, kh, vh = heads(r), heads(k), heads(v)
    y = y + jnp.sum(rh * kh * r_k, axis=-1, keepdims=True) * vh
    o_rwkv = (y.reshape(bsz, seqlen, RWKV_DIM) * g).astype(h.dtype)

    Cv = p[..., RWKV_COLS:]
    gate_b = Cv[..., :CONV_DIM]
    gate_c = Cv[..., CONV_DIM:2 * CONV_DIM]
    hc = Cv[..., 2 * CONV_DIM:]
    u = gate_c * hc
    u = conv_w[0] * shift_prev(u) + conv_w[1] * u + conv_w[2] * shift_next(u)
    o_conv = gate_b * u

    o_cat = jnp.concatenate([o_rwkv, o_conv], axis=-1) * out_scale
    return o_cat @ w_out


def clamped_swiglu(hid):
    glu = jnp.minimum(hid[..., :D_EXPERT], SWIGLU_LIMIT)
    lin = jnp.clip(hid[..., D_EXPERT:], -SWIGLU_LIMIT, SWIGLU_LIMIT)
    return glu * jax.nn.sigmoid(SWIGLU_ALPHA * glu) * (lin + 1.0)


def moe(x2d, w_router, b_router, w_exp_in, b_exp_in, w_exp_out, b_exp_out):
    T, D = x2d.shape
    logits = (x2d @ w_router + b_router).astype(jnp.float32)
    top_v, top_i = lax.top_k(logits, TOP_K)
    gates = jax.nn.softmax(top_v, axis=-1)

    A = T * TOP_K
    flat_e = top_i.reshape(-1).astype(jnp.int32)
    flat_tok = jnp.arange(A, dtype=jnp.int32) // TOP_K
    flat_g = gates.reshape(-1)
    order = jnp.argsort(flat_e)
    sorted_e = flat_e[order]
    counts = jnp.bincount(flat_e, length=N_EXPERTS)
    starts = jnp.cumsum(counts) - counts
    padded = (counts + MOE_BLOCK - 1) // MOE_BLOCK * MOE_BLOCK
    pends = jnp.cumsum(padded)
    pstarts = pends - padded
    dest = pstarts[sorted_e] + (jnp.arange(A, dtype=jnp.int32) - starts[sorted_e])

    n_blocks = -(-(A + N_EXPERTS * (MOE_BLOCK - 1)) // MOE_BLOCK)
    P = n_blocks * MOE_BLOCK
    row_tok = jnp.full((P,), T, jnp.int32).at[dest].set(flat_tok[order])
    row_gate = jnp.zeros((P,), jnp.float32).at[dest].set(flat_g[order])
    block_expert = jnp.clip(
        jnp.searchsorted(pends, jnp.arange(n_blocks, dtype=jnp.int32) * MOE_BLOCK, side='right'),
        0, N_EXPERTS - 1)

    x_pad = jnp.concatenate([x2d, jnp.zeros((1, D), x2d.dtype)], axis=0)
    xb = x_pad[row_tok].reshape(n_blocks, MOE_BLOCK, D)

    def expert_block(args):
        xblk, e = args
        hid = xblk @ w_exp_in[e] + b_exp_in[e]
        return clamped_swiglu(hid) @ w_exp_out[e] + b_exp_out[e]

    yb = lax.map(expert_block, (xb, block_expert))
    y_rows = yb.reshape(P, D).astype(jnp.float32) * row_gate[:, None]
    out = jax.ops.segment_sum(y_rows, row_tok, num_segments=T + 1)[:T]
    return out.astype(x2d.dtype)


def setup_inputs(seed: int = 0) -> dict:
    key = jax.random.key(seed)
    ks = jax.random.split(key, 32)
    f32 = jnp.float32
    nrm = lambda k, shape, s: jax.random.normal(k, shape, f32) * s
    L_ = DEPTH
    return {
        "x": nrm(ks[0], (BATCH, SEQ, D_MODEL), 1.0),
        "meta_tokens": nrm(ks[1], (N_META, D_MODEL), 1.0),
        "ln_in_g": 1.0 + nrm(ks[2], (D_MODEL,), 0.02),
        "ln_in_b": nrm(ks[3], (D_MODEL,), 0.02),
        "w_in": nrm(ks[4], (L_, D_MODEL, IN_COLS), D_MODEL ** -0.5),
        "mu_shift": jax.random.uniform(ks[5], (L_, RWKV_COLS), f32),
        "decay0": jax.random.uniform(ks[6], (L_, N_DIR, RWKV_DIM), f32, -6.0, -1.0),
        "decay_up": nrm(ks[7], (L_, N_DIR, DECAY_LORA, RWKV_DIM), 0.1 * DECAY_LORA ** -0.5),
        "iclr0": nrm(ks[8], (L_, N_DIR, RWKV_DIM), 0.5),
        "iclr_up": nrm(ks[9], (L_, N_DIR, ICLR_LORA, RWKV_DIM), 0.5 * ICLR_LORA ** -0.5),
        "gate_up": nrm(ks[10], (L_, GATE_LORA, RWKV_DIM), GATE_LORA ** -0.5),
        "k_k": 0.85 + nrm(ks[11], (L_, RWKV_DIM), 0.02),
        "k_a": 1.0 + nrm(ks[12], (L_, RWKV_DIM), 0.02),
        "r_k": nrm(ks[13], (L_, RWKV_HEADS, HEAD_SIZE), 0.1),
        "lnx_g": 1.0 + nrm(ks[14], (L_, RWKV_DIM), 0.02),
        "lnx_b": nrm(ks[15], (L_, RWKV_DIM), 0.02),
        "conv_w": nrm(ks[16], (L_, CONV_WIDTH, CONV_DIM), CONV_WIDTH ** -0.5),
        "out_scale": 1.0 + nrm(ks[17], (L_, MIX_WIDTH), 0.02),
        "w_out": nrm(ks[18], (L_, MIX_WIDTH, D_MODEL), BETA_INIT * MIX_WIDTH ** -0.5),
        "ln1_g": 1.0 + nrm(ks[19], (L_, D_MODEL), 0.02),
        "ln1_b": nrm(ks[20], (L_, D_MODEL), 0.02),
        "w_router": nrm(ks[21], (L_, D_MODEL, N_EXPERTS), D_MODEL ** -0.5),
        "b_router": nrm(ks[22], (L_, N_EXPERTS), 0.01),
        "w_exp_in": nrm(ks[23], (L_, N_EXPERTS, D_MODEL, 2 * D_EXPERT), D_MODEL ** -0.5),
        "b_exp_in": nrm(ks[24], (L_, N_EXPERTS, 2 * D_EXPERT), 0.02),
        "w_exp_out": nrm(ks[25], (L_, N_EXPERTS, D_EXPERT, D_MODEL), BETA_INIT * D_EXPERT ** -0.5),
        "b_exp_out": nrm(ks[26], (L_, N_EXPERTS, D_MODEL), 0.02),
        "ln2_g": 1.0 + nrm(ks[27], (L_, D_MODEL), 0.02),
        "ln2_b": nrm(ks[28], (L_, D_MODEL), 0.02),
    }


def reference(x, meta_tokens, ln_in_g, ln_in_b, w_in, mu_shift, decay0, decay_up, iclr0, iclr_up,
              gate_up, k_k, k_a, r_k, lnx_g, lnx_b, conv_w, out_scale, w_out, ln1_g, ln1_b,
              w_router, b_router, w_exp_in, b_exp_in, w_exp_out, b_exp_out, ln2_g, ln2_b):
    bsz = x.shape[0]
    meta = jnp.broadcast_to(meta_tokens[None].astype(x.dtype), (bsz, N_META, D_MODEL))
    h = jnp.concatenate([meta, x], axis=1)
    h = layer_norm(h, ln_in_g, ln_in_b)
    for l in range(DEPTH):
        m = mixer(h, w_in[l], mu_shift[l], decay0[l], decay_up[l], iclr0[l], iclr_up[l], gate_up[l],
                  k_k[l], k_a[l], r_k[l], lnx_g[l], lnx_b[l], conv_w[l], out_scale[l], w_out[l])
        h = layer_norm(ALPHA_RES * h + m, ln1_g[l], ln1_b[l])
        f = moe(h.reshape(-1, D_MODEL), w_router[l], b_router[l], w_exp_in[l], b_exp_in[l],
                w_exp_out[l], b_exp_out[l]).reshape(h.shape)
        h = layer_norm(ALPHA_RES * h + f, ln2_g[l], ln2_b[l])
    return h[:, N_META:]
```

```python
import numpy as np
from contextlib import ExitStack
import concourse.bass as bass
import concourse.mybir as mybir
from concourse.bass_utils import run_bass_kernel_spmd

F32 = mybir.dt.float32
BF16 = mybir.dt.bfloat16
AF = mybir.ActivationFunctionType
ALU = mybir.AluOpType
AX = mybir.AxisListType

NCORES = 8
D = 2048
T = 8208
TL = T // NCORES
KC = D // 128
CH = 64
NCH = 129
TP = NCH * CH
C0 = float(np.exp(-0.5))
LN_EPS = 1e-5
GN_EPS = 64e-5
ALPHA = float((2 * 2) ** 0.25)
NE = 32
EPC = NE // NCORES
FE = 2048
SEM_WRAP = 30000
DEBUG_TILES = None
DEBUG_STAGE = 0
DEBUG_MOE = None


class Obj:
    __slots__ = ("name", "w", "rs", "dsem", "dcnt")

    def __init__(self, name):
        self.name = name
        self.w = {}
        self.rs = {}
        self.dsem = None
        self.dcnt = 0


def _merge(dst, src):
    for k, (sem, val) in src.items():
        if k not in dst or dst[k][1] < val:
            dst[k] = (sem, val)


class Sched:
    def __init__(self, nc, st):
        self.nc = nc
        self.st = st
        self.eng = {"pe": nc.tensor, "dve": nc.vector, "act": nc.scalar, "pool": nc.gpsimd, "sp": nc.sync}
        self.gen = {k: 0 for k in self.eng}
        self.sem = {}
        self.cnt = {}
        for k in self.eng:
            self._newsem(k)
        self.seen = {k: {} for k in self.eng}
        self.nobj = 0

    def _newsem(self, e):
        self.sem[e] = self.st.enter_context(self.nc.semaphore("q%s%d" % (e, self.gen[e])))
        self.cnt[e] = 0
        self.gen[e] += 1

    def key(self, e):
        return "q_%s_%d" % (e, self.gen[e])

    def obj(self, name):
        self.nobj += 1
        return Obj("%s_%d" % (name, self.nobj))

    def _need(self, e, need):
        mykey = "q_%s_" % e
        for k, (sem, val) in need.items():
            if k.startswith(mykey) and e == "pe":
                continue
            if self.seen[e].get(k, 0) >= val:
                continue
            self.eng[e].wait_ge(sem, val)
            self.seen[e][k] = val

    def _collect(self, reads, writes):
        need = {}
        for o in reads:
            _merge(need, o.w)
        for o in writes:
            _merge(need, o.w)
            _merge(need, o.rs)
        return need

    def op(self, e, fn, reads=(), writes=(), signal=True):
        self._need(e, self._collect(reads, writes))
        ins = fn(self.eng[e])
        if signal:
            if self.cnt[e] >= SEM_WRAP:
                self._newsem(e)
            self.cnt[e] += 1
            ins.then_inc(self.sem[e], 1)
            tok = (self.sem[e], self.cnt[e])
        else:
            tok = (self.sem[e], self.cnt[e] + 1)
        k = self.key(e)
        for o in reads:
            _merge(o.rs, {k: tok})
        for o in writes:
            o.w = {k: tok}
            o.rs = {}
        return ins

    def dma(self, q, out, in_, sb, reads=(), writes=()):
        self._need(q, self._collect(reads, writes))
        if sb.dsem is None:
            sb.dsem = self.st.enter_context(self.nc.semaphore("d" + sb.name))
        sb.dcnt += 1
        ins = self.eng[q].dma_start(out=out, in_=in_)
        ins.then_inc(sb.dsem, 16)
        tok = (sb.dsem, 16 * sb.dcnt)
        k = "d_" + sb.name
        for o in reads:
            _merge(o.rs, {k: tok})
        for o in writes:
            neww = {kk: vv for kk, vv in o.w.items() if kk.startswith("d_")}
            neww[k] = tok
            o.w = neww
            o.rs = {}
        return ins

    def finish(self, objs, e="sp"):
        need = {}
        for o in objs:
            _merge(need, o.w)
        self._need(e, need)


class Ctx:
    def __init__(self, nc, st, S):
        self.nc, self.st, self.S = nc, st, S
        self.n = 0

    def sb(self, name, shape, dt):
        self.n += 1
        t = self.st.enter_context(self.nc.sbuf_tensor("%s_%d" % (name, self.n), list(shape), dt))
        return t, self.S.obj(name)

    def ps(self, name, shape, dt=F32):
        self.n += 1
        t = self.st.enter_context(self.nc.psum_tensor("%s_%d" % (name, self.n), list(shape), dt))
        return t, self.S.obj(name)


def TS(S, e, out, in0, s1, s2, op0, op1, R, W):
    if op1 is None:
        return S.op(e, lambda E: E.tensor_scalar(out=out, in0=in0, scalar1=s1, scalar2=None, op0=op0), R, W)
    return S.op(e, lambda E: E.tensor_scalar(out=out, in0=in0, scalar1=s1, scalar2=s2, op0=op0, op1=op1), R, W)


def TT(S, e, out, in0, in1, op, R, W):
    return S.op(e, lambda E: E.tensor_tensor(out=out, in0=in0, in1=in1, op=op), R, W)


def STT(S, out, in0, scalar, in1, op0, op1, R, W):
    return S.op("dve", lambda E: E.scalar_tensor_tensor(out=out, in0=in0, scalar=scalar, in1=in1, op0=op0, op1=op1), R, W)


def ACTF(S, out, in_, func, R, W, bias=None, scale=None):
    kw = {}
    if bias is not None:
        kw["bias"] = bias
    if scale is not None:
        kw["scale"] = scale
    return S.op("act", lambda E: E.activation(out=out, in_=in_, func=func, **kw), R, W)


def CP(S, e, out, in_, R, W):
    if e == "act":
        return S.op("act", lambda E: E.copy(out=out, in_=in_), R, W)
    return S.op(e, lambda E: E.tensor_copy(out=out, in_=in_), R, W)


def MM(S, out, lhsT, rhs, R, W, start=True, stop=True, signal=True):
    return S.op("pe", lambda E: E.matmul(out, lhsT=lhsT, rhs=rhs, start=start, stop=stop), R, W, signal=signal)


def TR(S, out, in_, ident, R, W, signal=True):
    return S.op("pe", lambda E: E.transpose(out, in_, ident), R, W, signal=signal)


def MSET(S, e, ap, val, W):
    return S.op(e, lambda E: E.memset(ap, val), (), W)


(SC_MU, SC_DEC0, SC_ICL0, SC_KK, SC_KA, SC_RK, SC_LG, SC_LB, SC_OSR, SC_CW, SC_OSC, SC_N) = (0, 6, 8, 10, 11, 12, 13, 14, 15, 16, 19, 20)


def mix_phase(nc, S, C, hT, w_in_c, sc_d, lora_d, cst_d, oT, hT_obj=None, oT_obj=None):
    st = C.st
    hT_obj = hT_obj or S.obj("hT")
    oT_obj = oT_obj or S.obj("oT")
    cst, o_cst = C.sb("cst", [128, 1024], F32)
    S.dma("sp", cst[:], cst_d[:, :], o_cst, (), (o_cst,))
    identb, o_identb = C.sb("identb", [128, 128], BF16)
    CP(S, "dve", identb[:], cst[:, 0:128], (o_cst,), (o_identb,))
    bones = cst[:, 128:256]
    masks = [cst[:, 256:576], cst[:, 576:896]]
    id64 = cst[:, 896:960]
    rmask = cst[:, 960:961]
    sc, o_sc = C.sb("sc", [128, SC_N + 2], F32)
    S.dma("sp", sc[:, 0:SC_N], sc_d[:, :], o_sc, (), (o_sc,))
    TS(S, "dve", sc[:, SC_N:SC_N + 1], sc[:, SC_KA:SC_KA + 1], -1.0, 1.0, ALU.mult, ALU.add, (o_sc,), (o_sc,))
    lo_f, o_lof = C.sb("lo_f", [128, 3, 128], F32)
    S.dma("sp", lo_f[:], lora_d[:, :, :], o_lof, (), (o_lof,))
    lo, o_lo = C.sb("lo", [128, 3, 128], BF16)
    CP(S, "dve", lo[:], lo_f[:], (o_lof,), (o_lo,))
    rst, o_rst = C.sb("rst", [128, 512], F32)
    MSET(S, "pool", rst[:], 1.0, (o_rst,))
    MSET(S, "pool", rst[:].rearrange("p (c j) -> p c j", j=CH)[:, :, 0:1], 0.0, (o_rst,))

    W_IN, o_win = C.sb("w_in", [128, KC, 1152], BF16)
    HB, o_hb = C.sb("hb", [128, KC, 514], BF16)
    HS = [C.sb("hs", [128, 514], F32) for _ in range(2)]
    PT = [C.sb("pt", [128, 514], F32) for _ in range(4)]
    for kc in range(KC):
        for q in range(3):
            t_, o_ = PT[(kc * 3 + q) % 4]
            S.dma("sp", t_[:, 0:384], w_in_c[kc * 128:(kc + 1) * 128, q * 384:(q + 1) * 384], o_, (), (o_,))
            CP(S, "dve" if q % 2 == 0 else "pool", W_IN[:, kc, q * 384:(q + 1) * 384], t_[:, 0:384], (o_,), (o_win,))
    R6, o_r6 = C.sb("r6", [128, 6, 512], F32)
    o_r6s = [S.obj("r6_%d" % j) for j in range(6)]
    YF, o_yf = C.sb("yf", [128, NCH, CH], BF16)

    def tmp(name, dt=F32, n=512):
        return C.sb(name, [128, n], dt)

    t_s, o_s = tmp("t_s"); t_d, o_d = tmp("t_d")
    t_kk0, o_kk0 = tmp("kk0"); t_sq, o_sq = t_s, o_s; t_nrm, o_nrm = tmp("nrm"); t_kk, o_kk = tmp("kk")
    t_tcw, o_tcw = tmp("tcw", BF16); t_cab, o_cab = tmp("cab", BF16)
    t_sg, o_sg = tmp("sg"); t_a, o_a = tmp("a"); t_t1, o_t1 = tmp("t1"); t_kd, o_kd = tmp("kd"); t_b, o_b = tmp("b")
    t_cum, o_cum = tmp("cum"); t_cumf, o_cumf = tmp("cumf"); t_te, o_te = t_kk0, o_kk0; t_cx, o_cx = t_d, o_d
    t_ep, o_ep = tmp("ep"); t_en, o_en = tmp("en"); t_ex, o_ex = tmp("ex"); t_ge, o_ge = tmp("ge")
    t_g, o_g = tmp("g"); t_bv, o_bv = tmp("bv"); t_of, o_of = tmp("of"); t_oo, o_oo = t_of, o_of
    t_u, o_u = tmp("u", F32, 514); t_cv, o_cv = tmp("cv"); t_oc, o_oc = t_cv, o_cv
    FM, o_fm = C.sb("fm", [128, 8, 7, CH], BF16)
    GR = [C.sb("gr", [128, 320], BF16) for _ in range(2)]
    TTt = [C.sb("ttt", [128, 320], BF16) for _ in range(2)]
    MMX = [C.sb("mmx", [128, 256], BF16) for _ in range(2)]
    GT, o_gt = C.sb("gt", [128, CH], BF16)
    QT, o_qt = C.sb("qt", [128, CH], BF16)
    HST = [C.sb("hst", [128, CH], BF16) for _ in range(2)]
    t_y, o_y = C.sb("y", [128, CH], F32)
    t_ysq, o_ysq = C.sb("ysq", [128, CH], F32)
    t_yn, o_yn = C.sb("yn", [128, CH], BF16)
    t_st, o_st = C.sb("bst", [128, 6], F32)
    t_mv, o_mv = C.sb("mv", [128, 2], F32)
    t_rs, o_rs = C.sb("rs", [128, 1], F32)

    PP = [C.ps("pp", [128, 2, 512])]
    PM, o_pm = C.ps("pm", [128, 512])
    BK3, o_pg = C.ps("bk3", [128, 512])
    BK4, o_ptr = C.ps("bk4", [128, 512])
    BK5, o_pn0 = C.ps("bk5", [128, 512])
    BK6, o_pn1 = C.ps("bk6", [128, 512])
    BK7, o_b7 = C.ps("bk7", [128, 512])
    PG = BK3[:, 0:320]
    PTR = BK4[:, 0:128].bitcast(BF16)
    PT2, o_pt2 = BK4[:, 128:160].bitcast(BF16), o_ptr
    PN = [(BK5[:, 0:256], o_pn0), (BK6[:, 0:256], o_pn1)]
    PAV, o_pav = BK7[:, 0:64], o_b7
    PGG, o_pgg = BK7[:, 64:128], o_b7
    PH, o_ph = BK7[:, 128:192], o_b7
    PQ, o_pq = BK7[:, 192:256], o_b7
    PY, o_py = BK7[:, 256:320], o_b7


    def bail():
        MSET(S, "dve", t_oo[:], 1.0, (o_oo,))
        S.dma("sp", oT[0:128, 0:512], t_oo[:, 0:512], o_oo, (o_oo,), (oT_obj,))
        return oT_obj
    ntiles = 17
    if DEBUG_STAGE == 1:
        return bail()
    cnt = {"hs": 0, "pt": 0, "pp": 0}

    def col(j):
        return sc[:, j:j + 1]

    for d in range(2):
        if DEBUG_STAGE == 13 and d == 1:
            return bail()
        hcur = 0
        MSET(S, "pool", HST[0][0][:], 0.0, (HST[0][1],))
        tiles = range(ntiles) if d == 0 else range(ntiles - 1, -1, -1)
        if DEBUG_TILES is not None:
            tiles = [t_ for t_ in tiles if t_ in DEBUG_TILES]
        ncols_w = 5 if d == 0 else 9
        for ti in tiles:
            t0 = ti * 512
            n = 512 if ti < 16 else CH
            nch = n // CH
            nreal = min(n, T - t0)
            lo_t, hi_t = max(t0 - 1, 0), min(t0 + n + 1, T)
            off = lo_t - (t0 - 1)
            ln = hi_t - lo_t
            clipped = (off != 0) or (ln != n + 2)
            for kc in range(KC):
                hs, o_hs = HS[cnt["hs"] % 2]
                cnt["hs"] += 1
                if clipped:
                    MSET(S, "pool", hs[:, 0:n + 2], 0.0, (o_hs,))
                S.dma("sp", hs[:, off:off + ln], hT[kc * 128:(kc + 1) * 128, lo_t:hi_t], o_hs,
                      (hT_obj,), (o_hs,))
                CP(S, "pool" if kc % 2 == 0 else "act", HB[:, kc, 0:n + 2], hs[:, 0:n + 2], (o_hs,), (o_hb,))
            if DEBUG_STAGE == 2:
                return bail()
            hw = (n + 2) // 2
            conv_pt = {}
            for j in range(ncols_w):
                pp, o_pp = PP[0]
                cnt["pp"] += 1
                for half in range(2):
                    for kc in range(KC):
                        MM(S, pp[:, half, 0:hw], W_IN[:, kc, j * 128:(j + 1) * 128], HB[:, kc, half * hw:(half + 1) * hw],
                           (o_win, o_hb), (o_pp,), start=(kc == 0), stop=(kc == KC - 1), signal=(kc == KC - 1))
                pt, o_pt = PT[cnt["pt"] % 4]
                cnt["pt"] += 1
                CP(S, "act", pt[:, 0:n + 2].rearrange("p (a b) -> p a b", a=2), pp[:, :, 0:hw], (o_pp,), (o_pt,))
                if j < 6:
                    pc = pt[:, 1:n + 1]
                    TT(S, "pool", t_s[:, 0:n], pt[:, 0:n], pt[:, 2:n + 2], ALU.add, (o_pt,), (o_s,))
                    STT(S, t_d[:, 0:n], t_s[:, 0:n], 0.5, pc, ALU.mult, ALU.subtract, (o_s, o_pt), (o_d,))
                    STT(S, R6[:, j, 0:n], t_d[:, 0:n], col(SC_MU + j), pc, ALU.mult, ALU.add, (o_d, o_pt, o_sc), (o_r6s[j],))
                    if nreal < n:
                        MSET(S, "dve", R6[:, j, nreal:n], 0.0, (o_r6s[j],))
                else:
                    conv_pt[j] = (pt, o_pt)
            if DEBUG_STAGE == 3:
                return bail()
            rr, kk_, vv, cw, ca, cg = (R6[:, j, 0:n] for j in range(6))
            o_r, o_k, o_v, o_cw, o_ca, o_cg = o_r6s
            TS(S, "pool", t_kk0[:, 0:n], kk_, col(SC_KK), None, ALU.mult, None, (o_k, o_sc), (o_kk0,))
            ACTF(S, t_sq[:, 0:n], t_kk0[:, 0:n], AF.Square, (o_kk0,), (o_sq,))
            MM(S, PM[:, 0:n], bones, t_sq[:, 0:n], (o_cst, o_sq), (o_pm,))
            ACTF(S, t_nrm[:, 0:n], PM[:, 0:n], AF.Sqrt, (o_pm,), (o_nrm,))
            TS(S, "dve", t_nrm[:, 0:n], t_nrm[:, 0:n], 1e-12, None, ALU.max, None, (o_nrm,), (o_nrm,))
            S.op("dve", lambda E: E.reciprocal(out=t_nrm[:, 0:n], in_=t_nrm[:, 0:n]), (o_nrm,), (o_nrm,))
            TT(S, "pool", t_kk[:, 0:n], t_kk0[:, 0:n], t_nrm[:, 0:n], ALU.mult, (o_kk0, o_nrm), (o_kk,))
            if DEBUG_STAGE == 4:
                return bail()
            ps_ = slice(64 * d, 64 * d + 64)
            ACTF(S, t_tcw[ps_, 0:n], R6[ps_, 3, 0:n], AF.Tanh, (o_cw,), (o_tcw,))
            MM(S, PM[:, 0:n], lo[ps_, 0, :], t_tcw[ps_, 0:n], (o_lo, o_tcw), (o_pm,))
            ACTF(S, t_sg[:, 0:n], PM[:, 0:n], AF.Sigmoid, (o_pm, o_sc), (o_sg,), bias=col(SC_DEC0 + d))
            CP(S, "pool", t_cab[ps_, 0:n], R6[ps_, 4, 0:n], (o_ca,), (o_cab,))
            MM(S, PM[:, 0:n], lo[ps_, 1, :], t_cab[ps_, 0:n], (o_lo, o_cab), (o_pm,))
            ACTF(S, t_a[:, 0:n], PM[:, 0:n], AF.Sigmoid, (o_pm, o_sc), (o_a,), bias=col(SC_ICL0 + d))
            TS(S, "dve", t_t1[:, 0:n], t_a[:, 0:n], col(SC_KA), col(SC_N), ALU.mult, ALU.add, (o_a, o_sc), (o_t1,))
            TT(S, "pool", t_kd[:, 0:n], kk_, t_t1[:, 0:n], ALU.mult, (o_k, o_t1), (o_kd,))
            TT(S, "pool", t_b[:, 0:n], t_kk[:, 0:n], t_a[:, 0:n], ALU.mult, (o_kk, o_a), (o_b,))
            S.op("dve", lambda E: E.tensor_tensor_scan(out=t_cumf[:, 0:n], data0=rst[:, 0:n], data1=t_sg[:, 0:n], initial=0.0,
                                                        op0=ALU.mult, op1=ALU.add), (o_rst, o_sg), (o_cumf,))
            v3 = lambda t_: t_[:, 0:n].rearrange("p (c j) -> p c j", j=CH)
            totb = v3(t_cumf)[:, :, CH - 1:CH].broadcast_to([128, nch, CH])
            if d == 0:
                cum, o_cumx = t_cumf, o_cumf
            else:
                TT(S, "dve", t_cum[:, 0:n], t_sg[:, 0:n], t_cumf[:, 0:n], ALU.subtract, (o_sg, o_cumf), (o_cum,))
                TT(S, "dve", v3(t_cum), v3(t_cum), totb, ALU.add, (o_cum, o_cumf), (o_cum,))
                cum, o_cumx = t_cum, o_cum
            TT(S, "dve", v3(t_te), totb, v3(cum), ALU.subtract, (o_cumf, o_cumx), (o_te,))
            TT(S, "pool", t_cx[:, 0:n], cum[:, 0:n], t_sg[:, 0:n], ALU.subtract, (o_cumx, o_sg), (o_cx,))
            ACTF(S, t_ep[:, 0:n], cum[:, 0:n], AF.Exp, (o_cumx,), (o_ep,), scale=-C0)
            ACTF(S, t_en[:, 0:n], cum[:, 0:n], AF.Exp, (o_cumx,), (o_en,), scale=C0)
            ACTF(S, t_ex[:, 0:n], t_cx[:, 0:n], AF.Exp, (o_cx,), (o_ex,), scale=-C0)
            ACTF(S, t_ge[:, 0:n], t_te[:, 0:n], AF.Exp, (o_te,), (o_ge,), scale=-C0)
            fm = lambda s_: FM[:, 0:nch, s_, :]
            STT(S, fm(0), v3(t_kk), -1.0, v3(t_ex), ALU.mult, ALU.mult, (o_kk, o_ex), (o_fm,))
            TT(S, "pool", fm(1), rr.rearrange("p (c j) -> p c j", j=CH), v3(t_ep), ALU.mult, (o_r, o_ep), (o_fm,))
            TT(S, "dve", fm(2), v3(t_b), v3(t_en), ALU.mult, (o_b, o_en), (o_fm,))
            TT(S, "pool", fm(3), v3(t_kd), v3(t_en), ALU.mult, (o_kd, o_en), (o_fm,))
            TT(S, "dve", fm(4), v3(t_b), v3(t_ge), ALU.mult, (o_b, o_ge), (o_fm,))
            TT(S, "pool", fm(5), v3(t_kd), v3(t_ge), ALU.mult, (o_kd, o_ge), (o_fm,))
            CP(S, "pool", fm(6), vv.rearrange("p (c j) -> p c j", j=CH), (o_v,), (o_fm,))
            if d == 1:
                ACTF(S, t_cab[:, 0:n], cg, AF.Sigmoid, (o_cg,), (o_cab,))
                MM(S, PM[:, 0:n], lo[:, 2, :], t_cab[:, 0:n], (o_lo, o_cab), (o_pm,))
                CP(S, "act", t_g[:, 0:n], PM[:, 0:n], (o_pm,), (o_g,))
                TT(S, "pool", t_s[:, 0:n], rr, kk_, ALU.mult, (o_r, o_k), (o_s,))
                TS(S, "dve", t_s[:, 0:n], t_s[:, 0:n], col(SC_RK), None, ALU.mult, None, (o_s, o_sc), (o_s,))
                MM(S, PM[:, 0:n], bones, t_s[:, 0:n], (o_cst, o_s), (o_pm,))
                TT(S, "dve", t_bv[:, 0:n], PM[:, 0:n], vv, ALU.mult, (o_pm, o_v), (o_bv,))
                (pgb, o_gb), (pgc, o_gc), (phc, o_hc) = conv_pt[6], conv_pt[7], conv_pt[8]
                TT(S, "pool", t_u[:, 0:n + 2], pgc[:, 0:n + 2], phc[:, 0:n + 2], ALU.mult, (o_gc, o_hc), (o_u,))
                TS(S, "dve", t_cv[:, 0:n], t_u[:, 0:n], col(SC_CW), None, ALU.mult, None, (o_u, o_sc), (o_cv,))
                STT(S, t_cv[:, 0:n], t_u[:, 1:n + 1], col(SC_CW + 1), t_cv[:, 0:n], ALU.mult, ALU.add, (o_u, o_sc, o_cv), (o_cv,))
                STT(S, t_cv[:, 0:n], t_u[:, 2:n + 2], col(SC_CW + 2), t_cv[:, 0:n], ALU.mult, ALU.add, (o_u, o_sc, o_cv), (o_cv,))
                STT(S, t_oc[:, 0:n], t_cv[:, 0:n], col(SC_OSC), pgb[:, 1:n + 1], ALU.mult, ALU.mult, (o_cv, o_sc, o_gb), (o_oc,))
                S.dma("sp", oT[128:256, t0:t0 + nreal], t_oc[:, 0:nreal], o_oc, (o_oc,), (oT_obj,))
            if DEBUG_STAGE == 5:
                return bail()
            if DEBUG_STAGE == 14 and d == 1:
                return bail()
            chunks = range(nch) if d == 0 else range(nch - 1, -1, -1)
            for c in chunks:
                gci = ti * 8 + c
                gr, o_gr = GR[gci % 2]
                tt_, o_tt = TTt[gci % 2]
                f = lambda s_, h: FM[64 * h:64 * h + 64, c, s_, :]
                f01 = lambda h: FM[64 * h:64 * h + 64, c, 0:2, :]
                for h in range(2):
                    hp = slice(64 * h, 64 * h + 64)
                    MM(S, PG[hp, 0:64], f(0, h), f(2, h), (o_fm,), (o_pg,), signal=False)
                    MM(S, PG[hp, 64:192], f(2, h), f01(h), (o_fm,), (o_pg,), signal=False)
                    MM(S, PG[hp, 192:320], f(3, h), f01(h), (o_fm,), (o_pg,), signal=(h == 1))
                TT(S, "dve", gr[:], PG[:], masks[d], ALU.mult, (o_pg, o_cst), (o_gr,))
                if DEBUG_STAGE == 6:
                    return bail()
                for h in range(2):
                    hp = slice(64 * h, 64 * h + 64)
                    for q, s_ in enumerate((4, 5, 6, 0)):
                        TR(S, PTR[hp, 64 * q:64 * q + 64], f(s_, h), identb[hp, hp], (o_fm, o_identb), (o_ptr,),
                           signal=(h == 1 and q == 3))
                CP(S, "act", tt_[:, 0:256], PTR[:], (o_ptr,), (o_tt,))
                if DEBUG_STAGE == 7:
                    return bail()
                for h in range(2):
                    hp = slice(64 * h, 64 * h + 64)
                    MM(S, PAV[hp, :], gr[hp, 192:256], tt_[hp, 128:192], (o_gr, o_tt), (o_pav,), signal=(h == 1))
                CP(S, "act", tt_[:, 256:320], PAV[:], (o_pav,), (o_tt,))
                if DEBUG_STAGE == 8:
                    return bail()
                for k in range(6):
                    if k == 0:
                        Msrc, MTsrc, Xsrc, o_src = gr[:, 0:64], gr[:, 64:128], tt_[:, 192:320], (o_gr, o_tt)
                    else:
                        mm_, o_mm = MMX[(k - 1) % 2]
                        Msrc, MTsrc, Xsrc, o_src = mm_[:, 0:64], mm_[:, 64:128], mm_[:, 128:256], (o_mm,)
                    pn, o_pn = PN[k % 2]
                    for h in range(2):
                        hp = slice(64 * h, 64 * h + 64)
                        if k < 5:
                            MM(S, pn[hp, 0:64], MTsrc[hp], Msrc[hp], o_src, (o_pn,), signal=False)
                            MM(S, pn[hp, 64:128], Msrc[hp], MTsrc[hp], o_src, (o_pn,), signal=False)
                        MM(S, pn[hp, 128:256], identb[hp, hp], Xsrc[hp], o_src + (o_identb,), (o_pn,), start=True, stop=False, signal=False)
                        MM(S, pn[hp, 128:256], MTsrc[hp], Xsrc[hp], o_src, (o_pn,), start=False, stop=True, signal=(h == 1))
                    mo, o_mo = MMX[k % 2]
                    lo_c = 0 if k < 5 else 128
                    CP(S, "dve" if k % 2 == 0 else "act", mo[:, lo_c:256], pn[:, lo_c:256], (o_pn,), (o_mo,))
                x6, o_x6 = MMX[5 % 2]
                if DEBUG_STAGE == 9:
                    return bail()
                Ah = lambda h: x6[64 * h:64 * h + 64, 128:192]
                Wh = lambda h: x6[64 * h:64 * h + 64, 192:256]
                for h in range(2):
                    hp = slice(64 * h, 64 * h + 64)
                    MM(S, PGG[hp, :], Ah(h), tt_[hp, 0:64], (o_x6, o_tt), (o_pgg,), signal=(h == 1))
                gcol = (CH - 1) if d == 0 else 0
                gam = t_ep[:, c * CH + gcol:c * CH + gcol + 1]
                STT(S, GT[:], id64, gam, PGG[:], ALU.mult, ALU.add, (o_cst, o_ep, o_pgg), (o_gt,))
                if DEBUG_STAGE == 10:
                    return bail()
                for h in range(2):
                    hp = slice(64 * h, 64 * h + 64)
                    MM(S, PQ[hp, :], Ah(h), gr[hp, 128:192], (o_x6, o_gr), (o_pq,), signal=(h == 1))
                TT(S, "dve", QT[:], PQ[:], FM[:, c, 1, :], ALU.add, (o_pq, o_fm), (o_qt,))
                if DEBUG_STAGE == 11:
                    return bail()
                hc_t, o_hc_ = HST[hcur]
                hn_t, o_hn_ = HST[1 - hcur]
                for h in range(2):
                    hp = slice(64 * h, 64 * h + 64)
                    MM(S, PY[hp, :], QT[hp, :], hc_t[hp, :], (o_qt, o_hc_), (o_py,), start=True, stop=False, signal=False)
                    MM(S, PY[hp, :], gr[hp, 128:192], Wh(h), (o_gr, o_x6), (o_py,), start=False, stop=False, signal=False)
                    MM(S, PY[hp, :], gr[hp, 256:320], tt_[hp, 128:192], (o_gr, o_tt), (o_py,), start=False, stop=True, signal=(h == 1))
                for h in range(2):
                    hp = slice(64 * h, 64 * h + 64)
                    MM(S, PH[hp, :], tt_[hp, 0:64], Wh(h), (o_tt, o_x6), (o_ph,), start=True, stop=False, signal=False)
                    MM(S, PH[hp, :], tt_[hp, 64:128], tt_[hp, 128:192], (o_tt,), (o_ph,), start=False, stop=False, signal=False)
                    MM(S, PH[hp, :], GT[hp, :], hc_t[hp, :], (o_gt, o_hc_), (o_ph,), start=False, stop=True, signal=(h == 1))
                CP(S, "act", hn_t[:], PH[:], (o_ph,), (o_hn_,))
                hcur = 1 - hcur
                if DEBUG_STAGE == 12:
                    return bail()
                if d == 0:
                    CP(S, "act", YF[:, gci, :], PY[:], (o_py,), (o_yf,))
                else:
                    if DEBUG_STAGE == 18:
                        return bail()
                    CP(S, "act", t_y[:], PY[:], (o_py,), (o_y,))
                    TT(S, "dve", t_y[:], t_y[:], YF[:, gci, :], ALU.add, (o_y, o_yf), (o_y,))
                    if DEBUG_STAGE == 19:
                        return bail()
                    S.op("dve", lambda E: E.reduce_sum(out=t_mv[:, 0:1], in_=t_y[:], axis=AX.X), (o_y,), (o_mv,))
                    TS(S, "dve", t_mv[:, 0:1], t_mv[:, 0:1], 1.0 / CH, None, ALU.mult, None, (o_mv,), (o_mv,))
                    TS(S, "dve", t_y[:], t_y[:], t_mv[:, 0:1], None, ALU.subtract, None, (o_y, o_mv), (o_y,))
                    TT(S, "dve", t_ysq[:], t_y[:], t_y[:], ALU.mult, (o_y,), (o_ysq,))
                    S.op("dve", lambda E: E.reduce_sum(out=t_rs[:], in_=t_ysq[:], axis=AX.X), (o_ysq,), (o_rs,))
                    TS(S, "dve", t_rs[:], t_rs[:], 1.0 / CH, GN_EPS, ALU.mult, ALU.add, (o_rs,), (o_rs,))
                    ACTF(S, t_rs[:], t_rs[:], AF.Sqrt, (o_rs,), (o_rs,))
                    S.op("dve", lambda E: E.reciprocal(out=t_rs[:], in_=t_rs[:]), (o_rs,), (o_rs,))
                    TS(S, "dve", t_yn[:], t_y[:], t_rs[:, 0:1], None, ALU.mult, None, (o_y, o_rs), (o_yn,))
                    if DEBUG_STAGE == 17:
                        return bail()
                    for h in range(2):
                        hp = slice(64 * h, 64 * h + 64)
                        TR(S, PT2[hp, :], t_yn[hp, :], identb[hp, hp], (o_yn, o_identb), (o_pt2,), signal=(h == 1))
                    ACTF(S, t_of[:, c * CH:(c + 1) * CH], PT2[:], AF.Identity, (o_pt2, o_sc), (o_of,),
                         bias=col(SC_LB), scale=col(SC_LG))
                    if DEBUG_STAGE == 15:
                        return bail()
            if d == 1:
                TT(S, "dve", t_of[:, 0:n], t_of[:, 0:n], t_bv[:, 0:n], ALU.add, (o_of, o_bv), (o_of,))
                TT(S, "pool", t_of[:, 0:n], t_of[:, 0:n], t_g[:, 0:n], ALU.mult, (o_of, o_g), (o_of,))
                TS(S, "dve", t_oo[:, 0:n], t_of[:, 0:n], col(SC_OSR), None, ALU.mult, None, (o_of, o_sc), (o_oo,))
                S.dma("sp", oT[0:128, t0:t0 + nreal], t_oo[:, 0:nreal], o_oo, (o_oo,), (oT_obj,))
    return oT_obj


def mix_consts():
    c = np.zeros((128, 1024), np.float32)
    c[:, 0:128] = np.eye(128, dtype=np.float32)
    c[0:64, 128:192] = 1.0
    c[64:128, 192:256] = 1.0
    i = np.arange(64)
    lt = (i[None, :] < i[:, None]).astype(np.float32)
    le = (i[None, :] <= i[:, None]).astype(np.float32)
    fwd = np.concatenate([lt, lt.T, le.T, lt.T, le.T], axis=1)
    bwd = np.concatenate([lt.T, lt, le, lt, le], axis=1)
    c[:, 256:576] = np.concatenate([fwd, fwd], axis=0)
    c[:, 576:896] = np.concatenate([bwd, bwd], axis=0)
    c[:, 896:960] = np.concatenate([np.eye(64), np.eye(64)], axis=0).astype(np.float32)
    return c


def mix_inputs(inp, l, c):
    RD = 1024
    w_in = inp["w_in"][l]
    hs = slice(128 * c, 128 * c + 128)
    cols = np.concatenate([
        np.arange(128 * c, 128 * c + 128), RD + np.arange(128 * c, 128 * c + 128), 2 * RD + np.arange(128 * c, 128 * c + 128),
        3 * RD + np.arange(0, 384),
        3456 + np.arange(128 * c, 128 * c + 128), 3456 + 1024 + np.arange(128 * c, 128 * c + 128),
        3456 + 2048 + np.arange(128 * c, 128 * c + 128)])
    w_c = np.ascontiguousarray(w_in[:, cols])
    sc = np.zeros((128, SC_N), np.float32)
    mu = inp["mu_shift"][l]
    sc[:, 0] = mu[0:RD][hs]; sc[:, 1] = mu[RD:2 * RD][hs]; sc[:, 2] = mu[2 * RD:3 * RD][hs]
    sc[:, 3] = mu[3 * RD:3 * RD + 128]; sc[:, 4] = mu[3 * RD + 128:3 * RD + 256]; sc[:, 5] = mu[3 * RD + 256:3 * RD + 384]
    for d in range(2):
        sc[:, SC_DEC0 + d] = inp["decay0"][l, d, hs]
        sc[:, SC_ICL0 + d] = inp["iclr0"][l, d, hs]
    sc[:, SC_KK] = inp["k_k"][l, hs]
    sc[:, SC_KA] = inp["k_a"][l, hs]
    sc[:, SC_RK] = inp["r_k"][l].reshape(-1)[hs]
    sc[:, SC_LG] = inp["lnx_g"][l, hs]
    sc[:, SC_LB] = inp["lnx_b"][l, hs]
    sc[:, SC_OSR] = inp["out_scale"][l, 0:RD][hs]
    for q in range(3):
        sc[:, SC_CW + q] = inp["conv_w"][l, q, hs]
    sc[:, SC_OSC] = inp["out_scale"][l, RD:][hs]
    lora = np.zeros((128, 3, 128), np.float32)
    lora[:, 0, :] = inp["decay_up"][l][:, :, hs].reshape(128, 128)
    lora[:, 1, :] = inp["iclr_up"][l][:, :, hs].reshape(128, 128)
    lora[:, 2, :] = inp["gate_up"][l][:, hs]
    return w_c, sc, lora


def build_mix():
    nc = bass.Bass("TRN2", target_bir_lowering=False)
    hT = nc.dram_tensor("hT", [D, T], F32, kind="ExternalInput").ap()
    w = nc.dram_tensor("w_in_c", [D, 1152], F32, kind="ExternalInput").ap()
    sc = nc.dram_tensor("sc", [128, SC_N], F32, kind="ExternalInput").ap()
    lora = nc.dram_tensor("lora", [128, 3, 128], F32, kind="ExternalInput").ap()
    cst = nc.dram_tensor("cst", [128, 1024], F32, kind="ExternalInput").ap()
    oT = nc.dram_tensor("oT", [256, T], F32, kind="ExternalOutput").ap()
    with ExitStack() as st:
        S = Sched(nc, st)
        C = Ctx(nc, st, S)
        o = mix_phase(nc, S, C, hT, w, sc, lora, cst, oT)
        S.finish([o])
    return nc


def run_mix(inp, l, hT_full):
    nc = build_mix()
    cst = mix_consts()
    maps = []
    for c in range(NCORES):
        w_c, sc, lora = mix_inputs(inp, l, c)
        maps.append({"hT": hT_full, "w_in_c": w_c, "sc": sc, "lora": lora, "cst": cst})
    res = run_bass_kernel_spmd(nc, maps, core_ids=list(range(NCORES)))
    return [r["oT"] for r in res.results]


class LNRes:
    pass


def ln_alloc(C, n):
    r = LNRes()
    r.sq = [C.sb("lnsq", [128, n], F32) for _ in range(2)]
    r.mean, r.o_mean = C.sb("lnmean", [128, n], F32)
    r.rstd, r.o_rstd = C.sb("lnrstd", [128, n], F32)
    r.msq, r.o_msq = C.sb("lnmsq", [128, n], F32)
    r.t1 = [C.sb("lnt1", [128, n], F32) for _ in range(2)]
    r.t2 = [C.sb("lnt2", [128, n], F32) for _ in range(2)]
    r.ps1, r.o_ps1 = C.ps("lnps1", [128, 512])
    r.ps2, r.o_ps2 = C.ps("lnps2", [128, 512])
    return r


def ln_fm(S, r, ones, o_ones, X, o_X, n, gb, o_gb, gcol, bcol, OUT, o_OUT):
    for kc in range(KC):
        sq, o_sq = r.sq[kc % 2]
        ACTF(S, sq[:, 0:n], X[:, kc, 0:n], AF.Square, (o_X,), (o_sq,))
        MM(S, r.ps1[:, 0:n], ones, X[:, kc, 0:n], (o_ones, o_X), (r.o_ps1,), start=(kc == 0), stop=(kc == KC - 1), signal=(kc == KC - 1))
        MM(S, r.ps2[:, 0:n], ones, sq[:, 0:n], (o_ones, o_sq), (r.o_ps2,), start=(kc == 0), stop=(kc == KC - 1), signal=True)
    ACTF(S, r.mean[:, 0:n], r.ps1[:, 0:n], AF.Copy, (r.o_ps1,), (r.o_mean,), scale=1.0 / D)
    TS(S, "dve", r.rstd[:, 0:n], r.ps2[:, 0:n], 1.0 / D, None, ALU.mult, None, (r.o_ps2,), (r.o_rstd,))
    TT(S, "pool", r.msq[:, 0:n], r.mean[:, 0:n], r.mean[:, 0:n], ALU.mult, (r.o_mean,), (r.o_msq,))
    TT(S, "dve", r.rstd[:, 0:n], r.rstd[:, 0:n], r.msq[:, 0:n], ALU.subtract, (r.o_rstd, r.o_msq), (r.o_rstd,))
    TS(S, "dve", r.rstd[:, 0:n], r.rstd[:, 0:n], LN_EPS, None, ALU.add, None, (r.o_rstd,), (r.o_rstd,))
    ACTF(S, r.rstd[:, 0:n], r.rstd[:, 0:n], AF.Sqrt, (r.o_rstd,), (r.o_rstd,))
    S.op("dve", lambda E: E.reciprocal(out=r.rstd[:, 0:n], in_=r.rstd[:, 0:n]), (r.o_rstd,), (r.o_rstd,))
    for kc in range(KC):
        t1, o_t1 = r.t1[kc % 2]
        t2, o_t2 = r.t2[kc % 2]
        TT(S, "dve", t1[:, 0:n], X[:, kc, 0:n], r.mean[:, 0:n], ALU.subtract, (o_X, r.o_mean), (o_t1,))
        TT(S, "pool", t2[:, 0:n], t1[:, 0:n], r.rstd[:, 0:n], ALU.mult, (o_t1, r.o_rstd), (o_t2,))
        ACTF(S, OUT[:, kc, 0:n], t2[:, 0:n], AF.Identity, (o_t2, o_gb), (o_OUT,),
             bias=gb[:, bcol + kc:bcol + kc + 1], scale=gb[:, gcol + kc:gcol + kc + 1])


NTT = 342


def load_fm(S, dram, d_obj, X, o_X, n_tok, col0=0):
    for kc in range(KC):
        S.dma("sp", X[:, kc, 0:n_tok], dram[kc * 128:(kc + 1) * 128, col0:col0 + n_tok], o_X, (d_obj,), (o_X,))


def store_fm(S, dram, d_obj, X, o_X, n_tok, col0=0):
    for kc in range(KC):
        S.dma("sp", dram[kc * 128:(kc + 1) * 128, col0:col0 + n_tok], X[:, kc, 0:n_tok], o_X, (o_X,), (d_obj,))


def ln0_phase(nc, S, C, xT, gb_d, hT_out, x_obj=None, h_obj=None):
    x_obj = x_obj or S.obj("xT")
    h_obj = h_obj or S.obj("h0T")
    ones, o_ones = C.sb("ones", [128, 128], F32)
    MSET(S, "pool", ones[:], 1.0, (o_ones,))
    gb, o_gb = C.sb("gb", [128, 32], F32)
    S.dma("sp", gb[:], gb_d[:, :], o_gb, (), (o_gb,))
    r = ln_alloc(C, NTT)
    X, o_X = C.sb("X", [128, KC, NTT], F32)
    Y, o_Y = C.sb("Y", [128, KC, NTT], F32)
    for tt in range(TL // NTT):
        load_fm(S, xT, x_obj, X, o_X, NTT, tt * NTT)
        ln_fm(S, r, ones[:], o_ones, X, o_X, NTT, gb, o_gb, 0, 16, Y, o_Y)
        store_fm(S, hT_out, h_obj, Y, o_Y, NTT, tt * NTT)
    return h_obj


def post1_phase(nc, S, C, ocT, hT, w_out_d, gb_d, wr_d, br_d, h1T_out, gates_out, objs=None):
    objs = objs or {}
    oc_obj = objs.get("oc") or S.obj("ocT")
    hin_obj = objs.get("h") or S.obj("hT")
    h1_obj = objs.get("h1") or S.obj("h1T")
    g_obj = objs.get("g") or S.obj("gates")
    ones, o_ones = C.sb("ones", [128, 128], F32)
    MSET(S, "pool", ones[:], 1.0, (o_ones,))
    gb, o_gb = C.sb("gb", [128, 32], F32)
    S.dma("sp", gb[:], gb_d[:, :], o_gb, (), (o_gb,))
    wr, o_wr = C.sb("wr", [128, KC, NE], F32)
    for kc in range(KC):
        S.dma("sp", wr[:, kc, :], wr_d[kc * 128:(kc + 1) * 128, :], o_wr, (), (o_wr,))
    br, o_br = C.sb("br", [1, NE], F32)
    S.dma("sp", br[:], br_d[:, :], o_br, (), (o_br,))
    WO, o_wo = C.sb("wo", [128, KC, D], BF16)
    wst = [C.sb("wst", [128, D], F32) for _ in range(2)]
    for kc in range(KC):
        t_, o_ = wst[kc % 2]
        S.dma("sp", t_[:], w_out_d[kc * 128:(kc + 1) * 128, :], o_, (), (o_,))
        CP(S, "dve" if kc % 2 == 0 else "pool", WO[:, kc, :], t_[:], (o_,), (o_wo,))
    r = ln_alloc(C, NTT)
    OC, o_OC = C.sb("OC", [128, KC, NTT], F32)
    OCB, o_OCB = C.sb("OCB", [128, KC, NTT], BF16)
    H, o_H = C.sb("H", [128, KC, NTT], F32)
    X1, o_X1 = C.sb("X1", [128, KC, NTT], F32)
    H1, o_H1 = C.sb("H1", [128, KC, NTT], F32)
    PSA = [C.ps("psa", [128, 512]) for _ in range(2)]
    PSR, o_psr = C.ps("psr", [128, 512])
    lg, o_lg = C.sb("lg", [128, NE], F32)
    mx, o_mx = C.sb("mx", [128, 8], F32)
    nmx, o_nmx = C.sb("nmx", [128, 1], F32)
    msk, o_msk = C.sb("msk", [128, NE], F32)
    ex, o_ex = C.sb("ex", [128, NE], F32)
    ssum, o_ssum = C.sb("ssum", [128, 1], F32)
    gt, o_gt = C.sb("gt", [128, NE], F32)
    for tt in range(TL // NTT):
        c0 = tt * NTT
        load_fm(S, ocT, oc_obj, OC, o_OC, NTT, c0)
        load_fm(S, hT, hin_obj, H, o_H, NTT, c0)
        for kc in range(KC):
            CP(S, "pool" if kc % 2 == 0 else "act", OCB[:, kc, :], OC[:, kc, :], (o_OC,), (o_OCB,))
        for oc in range(KC):
            ps, o_ps = PSA[oc % 2]
            for kc in range(KC):
                MM(S, ps[:, 0:NTT], WO[:, kc, oc * 128:(oc + 1) * 128], OCB[:, kc, :], (o_wo, o_OCB), (o_ps,),
                   start=(kc == 0), stop=(kc == KC - 1), signal=(kc == KC - 1))
            STT(S, X1[:, oc, :], H[:, oc, :], ALPHA, ps[:, 0:NTT], ALU.mult, ALU.add, (o_H, o_ps), (o_X1,))
        ln_fm(S, r, ones[:], o_ones, X1, o_X1, NTT, gb, o_gb, 0, 16, H1, o_H1)
        store_fm(S, h1T_out, h1_obj, H1, o_H1, NTT, c0)
        for s0 in range(0, NTT, 128):
            m = min(128, NTT - s0)
            for kc in range(KC):
                MM(S, PSR[0:m, 0:NE], H1[:, kc, s0:s0 + m], wr[:, kc, :], (o_H1, o_wr), (o_psr,), start=(kc == 0), stop=False, signal=False)
            MM(S, PSR[0:m, 0:NE], ones[0:1, 0:m], br[0:1, :], (o_ones, o_br), (o_psr,), start=False, stop=True)
            CP(S, "dve", lg[0:m, :], PSR[0:m, 0:NE], (o_psr,), (o_lg,))
            S.op("dve", lambda E: E.max(out=mx[0:m, :], in_=lg[0:m, :]), (o_lg,), (o_mx,))
            TS(S, "dve", msk[0:m, :], lg[0:m, :], mx[0:m, 3:4], None, ALU.is_ge, None, (o_lg, o_mx), (o_msk,))
            TS(S, "dve", nmx[0:m, :], mx[0:m, 0:1], -1.0, None, ALU.mult, None, (o_mx,), (o_nmx,))
            ACTF(S, ex[0:m, :], lg[0:m, :], AF.Exp, (o_lg, o_nmx), (o_ex,), bias=nmx[0:m, 0:1])
            TT(S, "dve", ex[0:m, :], ex[0:m, :], msk[0:m, :], ALU.mult, (o_ex, o_msk), (o_ex,))
            S.op("dve", lambda E: E.reduce_sum(out=ssum[0:m, :], in_=ex[0:m, :], axis=AX.X), (o_ex,), (o_ssum,))
            S.op("dve", lambda E: E.reciprocal(out=ssum[0:m, :], in_=ssum[0:m, :]), (o_ssum,), (o_ssum,))
            TS(S, "dve", gt[0:m, :], ex[0:m, :], ssum[0:m, 0:1], None, ALU.mult, None, (o_ex, o_ssum), (o_gt,))
            S.dma("sp", gates_out[c0 + s0:c0 + s0 + m, :], gt[0:m, :], o_gt, (o_gt,), (g_obj,))
    return h1_obj, g_obj


def post2_phase(nc, S, C, yparts, hT, gb_d, h2T_out, nparts, objs=None):
    objs = objs or {}
    y_obj = objs.get("y") or S.obj("yparts")
    hin_obj = objs.get("h") or S.obj("h1T")
    h2_obj = objs.get("h2") or S.obj("h2T")
    ones, o_ones = C.sb("ones", [128, 128], F32)
    MSET(S, "pool", ones[:], 1.0, (o_ones,))
    gb, o_gb = C.sb("gb", [128, 32], F32)
    S.dma("sp", gb[:], gb_d[:, :], o_gb, (), (o_gb,))
    r = ln_alloc(C, NTT)
    H, o_H = C.sb("H", [128, KC, NTT], F32)
    X1, o_X1 = C.sb("X1", [128, KC, NTT], F32)
    H2, o_H2 = C.sb("H2", [128, KC, NTT], F32)
    YP = [C.sb("YP", [128, KC, NTT], F32) for _ in range(2)]
    for tt in range(TL // NTT):
        c0 = tt * NTT
        load_fm(S, hT, hin_obj, H, o_H, NTT, c0)
        for p in range(nparts):
            yp, o_yp = YP[p % 2]
            load_fm(S, yparts[p], y_obj, yp, o_yp, NTT, c0)
            if p == 0:
                for kc in range(KC):
                    STT(S, X1[:, kc, :], H[:, kc, :], ALPHA, yp[:, kc, :], ALU.mult, ALU.add, (o_H, o_yp), (o_X1,))
            else:
                for kc in range(KC):
                    TT(S, "dve" if kc % 2 == 0 else "pool", X1[:, kc, :], X1[:, kc, :], yp[:, kc, :], ALU.add, (o_X1, o_yp), (o_X1,))
        ln_fm(S, r, ones[:], o_ones, X1, o_X1, NTT, gb, o_gb, 0, 16, H2, o_H2)
        store_fm(S, h2T_out, h2_obj, H2, o_H2, NTT, c0)
    return h2_obj


ST = TL
NST = T // ST


def moe_phase(nc, S, C, h1T, gT, win_d, bin_d, wout_d, bout_d, yT_out, objs=None):
    objs = objs or {}
    h_obj = objs.get("h") or S.obj("h1T")
    g_obj = objs.get("g") or S.obj("gT")
    y_obj = objs.get("y") or S.obj("yT")
    bi, o_bi = C.sb("bi", [128, EPC * 32], F32)
    S.dma("sp", bi[:], bin_d[:, :], o_bi, (), (o_bi,))
    HB, o_hb = C.sb("hb", [128, KC, ST], BF16)
    AC, o_ac = C.sb("ac", [128, KC, ST], BF16)
    Y, o_y = C.sb("y", [128, KC, ST], F32)
    hst = [C.sb("hst", [128, NTT], F32) for _ in range(2)]
    wst = [C.sb("wst", [128, KC * 128], F32) for _ in range(2)]
    wbf = [C.sb("wbf", [128, KC, 128], BF16) for _ in range(3)]
    bo, o_bo = C.sb("bo", [EPC, D], BF16)
    S.dma("sp", wst[0][0][0:EPC, :], bout_d[:, :], wst[0][1], (), (wst[0][1],))
    CP(S, "dve", bo[:], wst[0][0][0:EPC, :], (wst[0][1],), (o_bo,))
    G4, o_g4 = C.sb("g4", [EPC, ST], BF16)
    GB = [C.sb("gbc", [128, NTT], F32) for _ in range(2)]
    tg = [C.sb("tg", [128, NTT], F32) for _ in range(2)]
    tsg = [C.sb("tsg", [128, NTT], F32) for _ in range(2)]
    tl = [C.sb("tl", [128, NTT], F32) for _ in range(1)]
    PG_ = [C.ps("pg", [128, 512]) for _ in range(3)]
    PL_ = [C.ps("pl", [128, 512]) for _ in range(3)]
    PO_ = [C.ps("po", [128, 512]) for _ in range(2)]
    cn = {"wst": 0, "wbf": 0, "t": 0}
    ntt = ST // NTT

    def load_w(src2d):
        st_, o_st = wst[cn["wst"] % 2]
        cn["wst"] += 1
        S.dma("sp", st_[:], src2d, o_st, (), (o_st,))
        wb, o_wb = wbf[cn["wbf"] % 3]
        cn["wbf"] += 1
        CP(S, "pool", wb[:].rearrange("p a b -> p (a b)"), st_[:], (o_st,), (o_wb,))
        return wb, o_wb

    n_st, n_e = (NST, EPC) if DEBUG_MOE is None else DEBUG_MOE
    for s in range(n_st):
        t0 = s * ST
        q_ = 0
        for kc in range(KC):
            for tt in range(ST // NTT):
                hs_, o_hs = hst[q_ % 2]
                q_ += 1
                S.dma("sp", hs_[:], h1T[kc * 128:(kc + 1) * 128, t0 + tt * NTT:t0 + (tt + 1) * NTT], o_hs, (h_obj,), (o_hs,))
                CP(S, "act", HB[:, kc, tt * NTT:(tt + 1) * NTT], hs_[:], (o_hs,), (o_hb,))
        for tt in range(ST // NTT):
            hs_, o_hs = hst[q_ % 2]
            q_ += 1
            S.dma("sp", hs_[0:EPC, :], gT[:, t0 + tt * NTT:t0 + (tt + 1) * NTT], o_hs, (g_obj,), (o_hs,))
            CP(S, "act", G4[:, tt * NTT:(tt + 1) * NTT], hs_[0:EPC, :], (o_hs,), (o_g4,))
        for e in range(n_e):
            for fc in range(KC):
                wg, o_wg = load_w(win_d[e, fc])
                wl, o_wl = load_w(win_d[e, KC + fc])
                for tt in range(ntt):
                    q = cn["t"]
                    cn["t"] += 1
                    pg, o_pg = PG_[q % 3]
                    pl, o_pl = PL_[q % 3]
                    cs = slice(tt * NTT, (tt + 1) * NTT)
                    for kc in range(KC):
                        MM(S, pg[:, 0:NTT], wg[:, kc, :], HB[:, kc, cs], (o_wg, o_hb), (o_pg,), start=(kc == 0), stop=(kc == KC - 1), signal=(kc == KC - 1))
                    for kc in range(KC):
                        MM(S, pl[:, 0:NTT], wl[:, kc, :], HB[:, kc, cs], (o_wl, o_hb), (o_pl,), start=(kc == 0), stop=(kc == KC - 1), signal=(kc == KC - 1))
                    gb_, o_gb_ = GB[q % 2]
                    S.dma("sp", gb_[:], gT[e, t0 + tt * NTT:t0 + (tt + 1) * NTT].partition_broadcast(128), o_gb_, (g_obj,), (o_gb_,))
                    a_, o_a = tg[q % 2]
                    b_, o_b = tsg[q % 2]
                    c_, o_c = tl[0]
                    bg = bi[:, e * 32 + fc:e * 32 + fc + 1]
                    bl = bi[:, e * 32 + KC + fc:e * 32 + KC + fc + 1]
                    TS(S, "dve", a_[:], pg[:, 0:NTT], bg, 7.0, ALU.add, ALU.min, (o_pg, o_bi), (o_a,))
                    ACTF(S, b_[:], a_[:], AF.Sigmoid, (o_a,), (o_b,), scale=1.702)
                    TS(S, "dve", c_[:], pl[:, 0:NTT], bl, 7.0, ALU.add, ALU.min, (o_pl, o_bi), (o_c,))
                    TS(S, "pool", c_[:], c_[:], -7.0, 1.0, ALU.max, ALU.add, (o_c,), (o_c,))
                    TT(S, "pool", a_[:], a_[:], b_[:], ALU.mult, (o_a, o_b), (o_a,))
                    TT(S, "pool", a_[:], a_[:], c_[:], ALU.mult, (o_a, o_c), (o_a,))
                    TT(S, "dve", AC[:, fc, cs], a_[:], gb_[:], ALU.mult, (o_a, o_gb_), (o_ac,))
            for oc in range(KC):
                wo, o_wo = load_w(wout_d[e, oc])
                for tt in range(ntt):
                    q = cn["t"]
                    cn["t"] += 1
                    po, o_po = PO_[q % 2]
                    cs = slice(tt * NTT, (tt + 1) * NTT)
                    for fc in range(KC):
                        MM(S, po[:, 0:NTT], wo[:, fc, :], AC[:, fc, cs], (o_wo, o_ac), (o_po,), start=(fc == 0),
                           stop=(fc == KC - 1 and e != 0), signal=(fc == KC - 1 and e != 0))
                    if e == 0:
                        MM(S, po[:, 0:NTT], bo[0:EPC, oc * 128:(oc + 1) * 128], G4[0:EPC, cs], (o_bo, o_g4), (o_po,), start=False, stop=True)
                    if e == 0:
                        CP(S, "act", Y[:, oc, cs], po[:, 0:NTT], (o_po,), (o_y,))
                    else:
                        TT(S, "dve", Y[:, oc, cs], Y[:, oc, cs], po[:, 0:NTT], ALU.add, (o_y, o_po), (o_y,))
        for kc in range(KC):
            S.dma("sp", yT_out[kc * 128:(kc + 1) * 128, t0:t0 + ST], Y[:, kc, :], o_y, (o_y,), (y_obj,))
    return y_obj


def _tiles(cap):
    nt = -(-cap // 512)
    base = -(-cap // nt)
    base = -(-base // 2) * 2
    out, c0 = [], 0
    while c0 < cap:
        n = min(base, cap - c0)
        out.append((c0, n))
        c0 += n
    return out


def moe_sparse_phase(nc, S, C, hg, gg, win_d, bin_d, wout_d, bout_d, yg, CAP):
    h_obj, g_obj, y_obj = S.obj("hg"), S.obj("gg"), S.obj("yg")
    tiles = _tiles(CAP)
    NTM = max(n for _, n in tiles)
    bi, o_bi = C.sb("bi", [128, EPC * 32], F32)
    S.dma("sp", bi[:], bin_d[:, :], o_bi, (), (o_bi,))
    HB, o_hb = C.sb("hb", [128, KC, CAP], BF16)
    AC, o_ac = C.sb("ac", [128, KC, CAP], BF16)
    hst = [C.sb("hst", [128, NTM], F32) for _ in range(2)]
    wst = [C.sb("wst", [128, KC * 128], F32) for _ in range(2)]
    wbf = [C.sb("wbf", [128, KC, 128], BF16) for _ in range(3)]
    bo, o_bo = C.sb("bo", [1, D], BF16)
    G1, o_g1 = C.sb("g1", [1, CAP], BF16)
    GB = [C.sb("gbc", [128, NTM], F32) for _ in range(2)]
    tg = [C.sb("tg", [128, NTM], F32) for _ in range(2)]
    tsg = [C.sb("tsg", [128, NTM], F32) for _ in range(2)]
    tl = [C.sb("tl", [128, NTM], F32) for _ in range(2)]
    yo = [C.sb("yo", [128, NTM], F32) for _ in range(2)]
    PG_ = [C.ps("pg", [128, 512]) for _ in range(3)]
    PL_ = [C.ps("pl", [128, 512]) for _ in range(3)]
    PO_ = [C.ps("po", [128, 512]) for _ in range(2)]
    cn = {"wst": 0, "wbf": 0, "t": 0, "h": 0}

    def load_w(src2d):
        st_, o_st = wst[cn["wst"] % 2]
        cn["wst"] += 1
        S.dma("sp", st_[:], src2d, o_st, (), (o_st,))
        wb, o_wb = wbf[cn["wbf"] % 3]
        cn["wbf"] += 1
        CP(S, "pool", wb[:].rearrange("p a b -> p (a b)"), st_[:], (o_st,), (o_wb,))
        return wb, o_wb

    for e in range(EPC):
        for kc in range(KC):
            for (c0, n) in tiles:
                hs_, o_hs = hst[cn["h"] % 2]
                cn["h"] += 1
                S.dma("sp", hs_[:, 0:n], hg[e, kc * 128:(kc + 1) * 128, c0:c0 + n], o_hs, (h_obj,), (o_hs,))
                CP(S, "act", HB[:, kc, c0:c0 + n], hs_[:, 0:n], (o_hs,), (o_hb,))
        for (c0, n) in tiles:
            hs_, o_hs = hst[cn["h"] % 2]
            cn["h"] += 1
            S.dma("sp", hs_[0:1, 0:n], gg[e:e + 1, c0:c0 + n], o_hs, (g_obj,), (o_hs,))
            CP(S, "act", G1[0:1, c0:c0 + n], hs_[0:1, 0:n], (o_hs,), (o_g1,))
        st_, o_st = wst[cn["wst"] % 2]
        cn["wst"] += 1
        S.dma("sp", st_[0:1, 0:D], bout_d[e:e + 1, :], o_st, (), (o_st,))
        CP(S, "dve", bo[0:1, :], st_[0:1, 0:D], (o_st,), (o_bo,))
        for fc in range(KC):
            wg, o_wg = load_w(win_d[e, fc])
            wl, o_wl = load_w(win_d[e, KC + fc])
            for (c0, n) in tiles:
                q = cn["t"]
                cn["t"] += 1
                pg, o_pg = PG_[q % 3]
                pl, o_pl = PL_[q % 3]
                cs = slice(c0, c0 + n)
                for kc in range(KC):
                    MM(S, pg[:, 0:n], wg[:, kc, :], HB[:, kc, cs], (o_wg, o_hb), (o_pg,), start=(kc == 0), stop=(kc == KC - 1), signal=(kc == KC - 1))
                for kc in range(KC):
                    MM(S, pl[:, 0:n], wl[:, kc, :], HB[:, kc, cs], (o_wl, o_hb), (o_pl,), start=(kc == 0), stop=(kc == KC - 1), signal=(kc == KC - 1))
                gb_, o_gb_ = GB[q % 2]
                S.dma("sp", gb_[:, 0:n], gg[e, c0:c0 + n].partition_broadcast(128), o_gb_, (g_obj,), (o_gb_,))
                a_, o_a = tg[q % 2]
                b_, o_b = tsg[q % 2]
                c_, o_c = tl[q % 2]
                bg = bi[:, e * 32 + fc:e * 32 + fc + 1]
                bl = bi[:, e * 32 + KC + fc:e * 32 + KC + fc + 1]
                TS(S, "dve", a_[:, 0:n], pg[:, 0:n], bg, 7.0, ALU.add, ALU.min, (o_pg, o_bi), (o_a,))
                ACTF(S, b_[:, 0:n], a_[:, 0:n], AF.Sigmoid, (o_a,), (o_b,), scale=1.702)
                TS(S, "dve", c_[:, 0:n], pl[:, 0:n], bl, 7.0, ALU.add, ALU.min, (o_pl, o_bi), (o_c,))
                TS(S, "pool", c_[:, 0:n], c_[:, 0:n], -7.0, 1.0, ALU.max, ALU.add, (o_c,), (o_c,))
                TT(S, "pool", a_[:, 0:n], a_[:, 0:n], b_[:, 0:n], ALU.mult, (o_a, o_b), (o_a,))
                TT(S, "pool", a_[:, 0:n], a_[:, 0:n], c_[:, 0:n], ALU.mult, (o_a, o_c), (o_a,))
                TT(S, "dve", AC[:, fc, cs], a_[:, 0:n], gb_[:, 0:n], ALU.mult, (o_a, o_gb_), (o_ac,))
        for oc in range(KC):
            wo, o_wo = load_w(wout_d[e, oc])
            for (c0, n) in tiles:
                q = cn["t"]
                cn["t"] += 1
                po, o_po = PO_[q % 2]
                cs = slice(c0, c0 + n)
                for fc in range(KC):
                    MM(S, po[:, 0:n], wo[:, fc, :], AC[:, fc, cs], (o_wo, o_ac), (o_po,), start=(fc == 0), stop=False, signal=False)
                MM(S, po[:, 0:n], bo[0:1, oc * 128:(oc + 1) * 128], G1[0:1, cs], (o_bo, o_g1), (o_po,), start=False, stop=True)
                y_, o_y_ = yo[q % 2]
                CP(S, "act", y_[:, 0:n], po[:, 0:n], (o_po,), (o_y_,))
                S.dma("sp", yg[e, oc * 128:(oc + 1) * 128, c0:c0 + n], y_[:, 0:n], o_y_, (o_y_,), (y_obj,))
    return y_obj


def build_moe_sparse(CAP):
    def f(nc, S, C):
        h = _din(nc, "hg", [EPC, D, CAP]); g = _din(nc, "gg", [EPC, CAP])
        wi = _din(nc, "win", [EPC, 32, 128, KC * 128]); bi = _din(nc, "bin", [128, EPC * 32])
        wo = _din(nc, "wout", [EPC, KC, 128, KC * 128]); bo = _din(nc, "bout", [EPC, D])
        y = _dout(nc, "yg", [EPC, D, CAP])
        return [moe_sparse_phase(nc, S, C, h, g, wi, bi, wo, bo, y, CAP)]
    return _prog(f)


def moe_sparse_host(inp, l, h1T, gates):
    sel = gates > 0
    counts = sel.sum(0)
    CAP = int(-(-int(counts.max()) // 128) * 128)
    h1 = np.ascontiguousarray(h1T.T)
    idxs = [np.nonzero(sel[:, e])[0] for e in range(NE)]
    maps = []
    for c in range(NCORES):
        hg = np.zeros((EPC, D, CAP), np.float32)
        gg = np.zeros((EPC, CAP), np.float32)
        for k in range(EPC):
            e = EPC * c + k
            ix = idxs[e]
            hg[k, :, :len(ix)] = h1[ix].T
            gg[k, :len(ix)] = gates[ix, e]
        wi, bi, wo_e, bo = moe_inputs(inp, l, c)
        maps.append({"hg": hg, "gg": gg, "win": wi, "bin": bi, "wout": wo_e, "bout": bo})
    r = _run(build_moe_sparse(CAP), maps)
    slot = np.cumsum(sel, axis=1) - 1
    NS = int(sel.sum(1).max())
    ysel = np.zeros((NS, T, D), np.float32)
    for c in range(NCORES):
        yg = r[c]["yg"]
        for k in range(EPC):
            e = EPC * c + k
            ix = idxs[e]
            ye = np.ascontiguousarray(yg[k][:, :len(ix)].T)
            s = slot[ix, e]
            for q in range(NS):
                m = s == q
                if m.any():
                    ysel[q, ix[m]] = ye[m]
    return ysel, NS


def _prog(fn):
    nc = bass.Bass("TRN2", target_bir_lowering=False)
    with ExitStack() as st:
        S = Sched(nc, st)
        C = Ctx(nc, st, S)
        outs = fn(nc, S, C)
        S.finish(outs)
    return nc


def _din(nc, name, shape):
    return nc.dram_tensor(name, list(shape), F32, kind="ExternalInput").ap()


def _dout(nc, name, shape):
    return nc.dram_tensor(name, list(shape), F32, kind="ExternalOutput").ap()


def build_ln0():
    def f(nc, S, C):
        x = _din(nc, "xT", [D, TL]); gb = _din(nc, "gb", [128, 32]); o = _dout(nc, "hT", [D, TL])
        return [ln0_phase(nc, S, C, x, gb, o)]
    return _prog(f)


def build_post1():
    def f(nc, S, C):
        oc = _din(nc, "ocT", [D, TL]); h = _din(nc, "hT", [D, TL]); wo = _din(nc, "w_out", [D, D])
        gb = _din(nc, "gb", [128, 32]); wr = _din(nc, "wr", [D, NE]); br = _din(nc, "br", [1, NE])
        h1 = _dout(nc, "h1T", [D, TL]); g = _dout(nc, "gates", [TL, NE])
        return list(post1_phase(nc, S, C, oc, h, wo, gb, wr, br, h1, g))
    return _prog(f)


def build_post2(nparts=NCORES):
    def f(nc, S, C):
        y = _din(nc, "yparts", [nparts, D, TL]); h = _din(nc, "hT", [D, TL]); gb = _din(nc, "gb", [128, 32])
        o = _dout(nc, "h2T", [D, TL])
        return [post2_phase(nc, S, C, y, h, gb, o, nparts)]
    return _prog(f)


def build_moe():
    def f(nc, S, C):
        h = _din(nc, "h1T", [D, T]); g = _din(nc, "gT", [EPC, T])
        wi = _din(nc, "win", [EPC, 32, 128, KC * 128]); bi = _din(nc, "bin", [128, EPC * 32])
        wo = _din(nc, "wout", [EPC, KC, 128, KC * 128]); bo = _din(nc, "bout", [EPC, D])
        y = _dout(nc, "yT", [D, T])
        return [moe_phase(nc, S, C, h, g, wi, bi, wo, bo, y)]
    return _prog(f)


def _gb(g, b):
    return np.ascontiguousarray(np.concatenate([g.reshape(KC, 128).T, b.reshape(KC, 128).T], axis=1).astype(np.float32))


def _run(nc, maps):
    res = run_bass_kernel_spmd(nc, maps, core_ids=list(range(len(maps))))
    return res.results


def moe_inputs(inp, l, c):
    es = slice(EPC * c, EPC * c + EPC)
    wi = inp["w_exp_in"][l, es]
    wi = wi.reshape(EPC, KC, 128, 32, 128).transpose(0, 3, 2, 1, 4).reshape(EPC, 32, 128, KC * 128)
    wo = inp["w_exp_out"][l, es]
    wo = wo.reshape(EPC, KC, 128, KC, 128).transpose(0, 3, 2, 1, 4).reshape(EPC, KC, 128, KC * 128)
    bi = inp["b_exp_in"][l, es].reshape(EPC, 32, 128).transpose(2, 0, 1).reshape(128, EPC * 32)
    bo = inp["b_exp_out"][l, es]
    return (np.ascontiguousarray(wi), np.ascontiguousarray(bi), np.ascontiguousarray(wo), np.ascontiguousarray(bo))


def w_out_perm(inp, l):
    idx = np.concatenate([np.concatenate([np.arange(128 * c, 128 * c + 128), 1024 + np.arange(128 * c, 128 * c + 128)]) for c in range(NCORES)])
    return np.ascontiguousarray(inp["w_out"][l][idx, :])


def kernel(**inp):
    inp = {k: np.asarray(v) for k, v in inp.items()}
    x = inp["x"][0]
    full = np.concatenate([inp["meta_tokens"].astype(np.float32), x], axis=0)
    fullT = np.ascontiguousarray(full.T)
    sl = lambda c: slice(c * TL, (c + 1) * TL)
    nc = build_ln0()
    gb = _gb(inp["ln_in_g"], inp["ln_in_b"])
    r = _run(nc, [{"xT": np.ascontiguousarray(fullT[:, sl(c)]), "gb": gb} for c in range(NCORES)])
    hT = np.concatenate([r[c]["hT"] for c in range(NCORES)], axis=1)
    nc_mix = build_mix()
    nc_p1 = build_post1()
    cst = mix_consts()
    for l in range(2):
        maps = []
        for c in range(NCORES):
            w_c, sc, lora = mix_inputs(inp, l, c)
            maps.append({"hT": hT, "w_in_c": w_c, "sc": sc, "lora": lora, "cst": cst})
        r = _run(nc_mix, maps)
        ocT = np.concatenate([r[c]["oT"] for c in range(NCORES)], axis=0)
        wo = w_out_perm(inp, l)
        gb1 = _gb(inp["ln1_g"][l], inp["ln1_b"][l])
        wr = np.ascontiguousarray(inp["w_router"][l]); br = np.ascontiguousarray(inp["b_router"][l][None, :])
        r = _run(nc_p1, [{"ocT": np.ascontiguousarray(ocT[:, sl(c)]), "hT": np.ascontiguousarray(hT[:, sl(c)]), "w_out": wo,
                          "gb": gb1, "wr": wr, "br": br} for c in range(NCORES)])
        h1T = np.concatenate([r[c]["h1T"] for c in range(NCORES)], axis=1)
        gates = np.concatenate([r[c]["gates"] for c in range(NCORES)], axis=0)
        gT = np.ascontiguousarray(gates.T)
        ysel, NS = moe_sparse_host(inp, l, h1T, gates)
        gb2 = _gb(inp["ln2_g"][l], inp["ln2_b"][l])
        nc_p2 = build_post2(NS)
        maps = []
        for c in range(NCORES):
            yp = np.ascontiguousarray(ysel[:, sl(c), :].transpose(0, 2, 1))
            maps.append({"yparts": yp, "hT": np.ascontiguousarray(h1T[:, sl(c)]), "gb": gb2})
        r = _run(nc_p2, maps)
        hT = np.concatenate([r[c]["h2T"] for c in range(NCORES)], axis=1)
    out = np.ascontiguousarray(hT.T[16:])[None]
    return out.astype(np.float32)
```
